# Optimizing a Trainium2 kernel written in Bass

```python
import jax, jax.numpy as jnp
from jax import lax
import numpy as np

D_MODEL = 4096
BATCH = 4
SEQ = 2048
DEPTH = 4

N_MIXERS = 3
D_FF = 5632
RET_HEADS = 16
RET_DK = D_MODEL // RET_HEADS
RET_DV = D_MODEL // RET_HEADS
RET_CHUNK = 128
SG_GROUPS = 16
SG_CHUNK = 128
MOBA_HEADS = 32
MOBA_DH = D_MODEL // MOBA_HEADS
MOBA_BLOCK = 256
MOBA_TOPK = 3
MOBA_QBLOCK = 8
ROPE_THETA = 10000.0
EPS = 1e-6

kernel_name = "hybrid_retention_sgu_moba_macaron"


def _n_layers_of(m):
    return len(range(m, DEPTH, N_MIXERS))


def rms_norm(x, g):
    xf = x.astype(jnp.float32)
    y = xf * lax.rsqrt(jnp.mean(xf * xf, axis=-1, keepdims=True) + EPS)
    return (y * g.astype(jnp.float32)).astype(x.dtype)


def layer_norm(x, g):
    xf = x.astype(jnp.float32)
    mu = jnp.mean(xf, axis=-1, keepdims=True)
    var = jnp.mean(jnp.square(xf - mu), axis=-1, keepdims=True)
    return ((xf - mu) * lax.rsqrt(var + EPS) * g.astype(jnp.float32)).astype(x.dtype)


def swiglu(x, w_in, w_out):
    a, b = jnp.split(x @ w_in, 2, axis=-1)
    return (jax.nn.silu(a) * b) @ w_out


def rotary(t, positions, inv_freq):
    ang = positions[:, None, :, None].astype(jnp.float32) * inv_freq
    cos = jnp.cos(ang).astype(t.dtype)
    sin = jnp.sin(ang).astype(t.dtype)
    t1, t2 = jnp.split(t, 2, axis=-1)
    return jnp.concatenate([t1 * cos - t2 * sin, t1 * sin + t2 * cos], axis=-1)


def retention_mixer(xn, positions, w_in, gn_g, w_out):
    B_, S, D = xn.shape
    H, dk, dv, C = RET_HEADS, RET_DK, RET_DV, RET_CHUNK
    N = S // C
    dt = xn.dtype
    q, k, v, g = jnp.split(xn @ w_in, 4, axis=-1)
    heads = lambda t, d: t.reshape(B_, S, H, d).transpose(0, 2, 1, 3)
    inv_freq = 1.0 / (10000.0 ** jnp.linspace(0.0, 1.0, dk // 2, dtype=jnp.float32))
    q = rotary(heads(q, dk), positions, inv_freq)
    k = rotary(heads(k, dk), positions, inv_freq) * (dk ** -0.5)
    v = heads(v, dv)
    log_gamma = jnp.log1p(-jnp.exp2(-5.0 - jnp.arange(H, dtype=jnp.float32)))
    idx = jnp.arange(C, dtype=jnp.float32)
    dist = idx[:, None] - idx[None, :]
    intra_decay = jnp.where(dist >= 0,
                            jnp.exp(log_gamma[:, None, None] * jnp.maximum(dist, 0.0)),
                            0.0).astype(dt)
    q_decay = jnp.exp(log_gamma[:, None] * (idx + 1.0)).astype(dt)
    k_decay = jnp.exp(log_gamma[:, None] * (C - 1.0 - idx)).astype(dt)
    chunk_decay = jnp.exp(log_gamma * C).astype(dt)
    qc = q.reshape(B_, H, N, C, dk)
    kc = k.reshape(B_, H, N, C, dk)
    vc = v.reshape(B_, H, N, C, dv)
    scores = jnp.einsum('bhncd,bhnmd->bhncm', qc, kc) * intra_decay[:, None]
    intra = jnp.einsum('bhncm,bhnme->bhnce', scores, vc)

    def step(state, inp):
        q_n, k_n, v_n = inp
        cross = jnp.einsum('bhcd,bhde->bhce', q_n * q_decay[:, :, None], state)
        state = state * chunk_decay[:, None, None] + jnp.einsum(
            'bhcd,bhce->bhde', k_n * k_decay[:, :, None], v_n)
        return state, cross

    state0 = jnp.zeros((B_, H, dk, dv), dt)
    to_chunk_major = lambda t: t.transpose(2, 0, 1, 3, 4)
    _, cross = lax.scan(step, state0, (to_chunk_major(qc), to_chunk_major(kc), to_chunk_major(vc)))
    o = (intra + cross.transpose(1, 2, 0, 3, 4)).reshape(B_, H, S, dv)
    of = o.astype(jnp.float32)
    mu = jnp.mean(of, axis=-1, keepdims=True)
    var = jnp.mean(jnp.square(of - mu), axis=-1, keepdims=True)
    o = ((of - mu) * lax.rsqrt(var + EPS)).astype(dt)
    o = o.transpose(0, 2, 1, 3).reshape(B_, S, H * dv) * gn_g
    return (jax.nn.silu(g) * o) @ w_out


def spatial_gating_mixer(xn, w_in, ln_g, w_s, b_s, w_out):
    B_, S, D = xn.shape
    G, C = SG_GROUPS, SG_CHUNK
    N = S // C
    u, v = jnp.split(jax.nn.gelu(xn @ w_in, approximate=False), 2, axis=-1)
    v = layer_norm(v, ln_g).reshape(B_, N, C, G, D // G)
    causal = jnp.tril(jnp.ones((C, C), dtype=bool))
    w_m = jnp.where(causal[None], w_s, 0.0)
    mixed = jnp.einsum('gts,bnsgc->bntgc', w_m, v) + b_s.T[None, None, :, :, None]
    return (u * mixed.reshape(B_, S, D)) @ w_out


def moba_mixer(xn, positions, w_in, w_out):
    B_, S, D = xn.shape
    H, dh, L, QB = MOBA_HEADS, MOBA_DH, MOBA_BLOCK, MOBA_QBLOCK
    nblk = -(-S // L)
    Sp = nblk * L
    n_sel = min(MOBA_TOPK, nblk - 1)
    scale = dh ** -0.5
    q, k, v = jnp.split(xn @ w_in, 3, axis=-1)
    heads = lambda t: t.reshape(B_, S, H, dh).transpose(0, 2, 1, 3)
    inv_freq = 1.0 / (ROPE_THETA ** (jnp.arange(0, dh, 2, dtype=jnp.float32) / dh))
    q = rotary(heads(q), positions, inv_freq)
    k = rotary(heads(k), positions, inv_freq)
    v = heads(v)
    pad = ((0, 0), (0, 0), (0, Sp - S), (0, 0))
    kb = jnp.pad(k, pad).reshape(B_, H, nblk, L, dh)
    vb = jnp.pad(v, pad).reshape(B_, H, nblk, L, dh)
    q_blk = jnp.arange(S) // L
    nq = S // QB
    q_steps = q.reshape(B_, H, nq, QB, dh).transpose(2, 0, 1, 3, 4)
    t0s = jnp.arange(nq, dtype=jnp.int32) * QB

    if n_sel > 0:
        k_mean = jnp.mean(kb, axis=3)
        gate = jnp.einsum('bhsd,bhnd->bhsn', q, k_mean).astype(jnp.float32)
        past = jnp.arange(nblk)[None, :] < q_blk[:, None]
        gate = jnp.where(past, gate, -jnp.inf)
        _, sel = lax.top_k(gate, n_sel)
        valid = sel < q_blk[None, None, :, None]
        sel_steps = sel.reshape(B_, H, nq, QB, n_sel).transpose(2, 0, 1, 3, 4)
        valid_steps = valid.reshape(B_, H, nq, QB, n_sel).transpose(2, 0, 1, 3, 4)
    else:
        sel_steps = jnp.zeros((nq, B_, H, QB, 1), jnp.int32)
        valid_steps = jnp.zeros((nq, B_, H, QB, 1), bool)

    bi = jnp.arange(B_)[:, None, None, None]
    hi = jnp.arange(H)[None, :, None, None]

    def attend(args):
        q_i, sel_i, valid_i, t0 = args
        own = t0 // L
        k_own = lax.dynamic_index_in_dim(kb, own, axis=2, keepdims=False)
        v_own = lax.dynamic_index_in_dim(vb, own, axis=2, keepdims=False)
        qpos = t0 + jnp.arange(QB)
        kpos = own * L + jnp.arange(L)
        s_own = jnp.einsum('bhqd,bhkd->bhqk', q_i, k_own).astype(jnp.float32) * scale
        s_own = jnp.where(kpos[None, :] <= qpos[:, None], s_own, -jnp.inf)
        if n_sel > 0:
            k_sel = kb[bi, hi, sel_i]
            v_sel = vb[bi, hi, sel_i]
            s_sel = jnp.einsum('bhqd,bhqnkd->bhqnk', q_i, k_sel).astype(jnp.float32) * scale
            s_sel = jnp.where(valid_i[..., None], s_sel, -jnp.inf).reshape(B_, H, QB, n_sel * L)
            p = jax.nn.softmax(jnp.concatenate([s_sel, s_own], axis=-1), axis=-1).astype(v.dtype)
            p_sel = p[..., :n_sel * L].reshape(B_, H, QB, n_sel, L)
            p_own = p[..., n_sel * L:]
            return (jnp.einsum('bhqnk,bhqnkd->bhqd', p_sel, v_sel)
                    + jnp.einsum('bhqk,bhkd->bhqd', p_own, v_own))
        p_own = jax.nn.softmax(s_own, axis=-1).astype(v.dtype)
        return jnp.einsum('bhqk,bhkd->bhqd', p_own, v_own)

    out = lax.map(attend, (q_steps, sel_steps, valid_steps, t0s))
    out = out.transpose(1, 0, 3, 2, 4).reshape(B_, S, H * dh)
    return out @ w_out


def setup_inputs(seed: int = 0) -> dict:
    key = jax.random.key(seed)
    ks = jax.random.split(key, 16)
    D = D_MODEL
    n_ret, n_sg, n_mo = _n_layers_of(0), _n_layers_of(1), _n_layers_of(2)
    nrm = lambda k, shape, fan_in: jax.random.normal(k, shape, jnp.float32) * (fan_in ** -0.5)
    gain = lambda k, shape: 1.0 + 0.05 * jax.random.normal(k, shape, jnp.float32)
    x = jax.random.normal(ks[0], (BATCH, SEQ, D), jnp.float32)
    positions = jnp.broadcast_to(jnp.arange(SEQ, dtype=jnp.int32), (BATCH, SEQ))
    return {
        "x": x,
        "positions": positions,
        "norm_g": gain(ks[1], (DEPTH, 6, D)),
        "ffn_w_in": nrm(ks[2], (DEPTH, 2, D, 2 * D_FF), D),
        "ffn_w_out": nrm(ks[3], (DEPTH, 2, D_FF, D), D_FF),
        "ret_w_in": nrm(ks[4], (n_ret, D, 4 * D), D),
        "ret_gn_g": gain(ks[5], (n_ret, D)),
        "ret_w_out": nrm(ks[6], (n_ret, D, D), D),
        "sg_w_in": nrm(ks[7], (n_sg, D, 2 * D), D),
        "sg_ln_g": gain(ks[8], (n_sg, D)),
        "sg_w_s": nrm(ks[9], (n_sg, SG_GROUPS, SG_CHUNK, SG_CHUNK), SG_CHUNK),
        "sg_b": 1.0 + 0.1 * jax.random.normal(ks[10], (n_sg, SG_GROUPS, SG_CHUNK), jnp.float32),
        "sg_w_out": nrm(ks[11], (n_sg, D, D), D),
        "moba_w_in": nrm(ks[12], (n_mo, D, 3 * D), D),
        "moba_w_out": nrm(ks[13], (n_mo, D, D), D),
    }


def reference(x, positions, norm_g, ffn_w_in, ffn_w_out, ret_w_in, ret_gn_g, ret_w_out,
              sg_w_in, sg_ln_g, sg_w_s, sg_b, sg_w_out, moba_w_in, moba_w_out):
    h = x
    for i in range(DEPTH):
        m, j = i % N_MIXERS, i // N_MIXERS
        g = norm_g[i]
        h = h + 0.5 * rms_norm(swiglu(rms_norm(h, g[0]), ffn_w_in[i, 0], ffn_w_out[i, 0]), g[1])
        hn = rms_norm(h, g[2])
        if m == 0:
            y = retention_mixer(hn, positions, ret_w_in[j], ret_gn_g[j], ret_w_out[j])
        elif m == 1:
            y = spatial_gating_mixer(hn, sg_w_in[j], sg_ln_g[j], sg_w_s[j], sg_b[j], sg_w_out[j])
        else:
            y = moba_mixer(hn, positions, moba_w_in[j], moba_w_out[j])
        h = h + rms_norm(y, g[3])
        h = h + 0.5 * rms_norm(swiglu(rms_norm(h, g[4]), ffn_w_in[i, 1], ffn_w_out[i, 1]), g[5])
    return h
```

```python
import os
import numpy as np
import concourse.bass as bass
import concourse.mybir as mybir
from concourse.bass_utils import run_bass_kernel_spmd

F32 = mybir.dt.float32
BF16 = mybir.dt.bfloat16
I32 = mybir.dt.int32
AF = mybir.ActivationFunctionType
ALU = mybir.AluOpType
AX = mybir.AxisListType

D = 4096
KC = 32
T = 2048
TG = 512
NTG = T // TG
DFF = 5632
HC = DFF // 128
EPS = 1e-6
NEG = -1.0e30
TWO_PI = float(2.0 * np.pi)
PI = float(np.pi)

_DBG_LAYERS = os.environ.get("K_LAYERS")
_DBG_STAGES = os.environ.get("K_STAGES")


class Buf:
    __slots__ = ("w", "r")

    def __init__(self):
        self.w = None
        self.r = {}


class Q:
    def __init__(self, h, key, is_pe=False, dkeys=()):
        self.h = h
        self.key = key
        self.count = 0
        self.waited = {}
        self.is_pe = is_pe
        self.dkeys = list(dkeys)
        self.dnext = 0


class WT:
    def __init__(self, ap, buf, kc, nb, cb):
        self.ap, self.buf, self.kc, self.nb, self.cb = ap, buf, kc, nb, cb


class Prog:
    def __init__(self, nc, sems):
        self.nc = nc
        self.sems = sems
        self.PE = Q(nc.tensor, "pe", is_pe=True)
        self.ACT = Q(nc.scalar, "act")
        self.DVE = Q(nc.vector, "dve")
        self.SP = Q(nc.sync, "sp", dkeys=["sd%d" % i for i in range(16)])
        self.PQ = Q(nc.gpsimd, "pool", dkeys=["pd%d" % i for i in range(12)])
        self.allq = [self.PE, self.ACT, self.DVE, self.SP, self.PQ]
        self.dcount = {}
        for q in (self.SP, self.PQ):
            for k in q.dkeys:
                self.dcount[k] = 0
        self.cccount = {}

    def _wait(self, q, ev):
        k, v = ev
        if v <= 0:
            return
        if k == q.key and q.is_pe:
            return
        if q.waited.get(k, 0) >= v:
            return
        q.h.wait_ge(self.sems[k], v)
        q.waited[k] = v

    def _deps(self, q, reads, writes):
        for b in reads:
            if b.w is not None:
                self._wait(q, b.w)
        for b in writes:
            if b.w is not None:
                self._wait(q, b.w)
            for k, v in b.r.items():
                self._wait(q, (k, v))

    def _mark(self, ev, reads, writes):
        for b in reads:
            if b.r.get(ev[0], 0) < ev[1]:
                b.r[ev[0]] = ev[1]
        for b in writes:
            b.w = ev
            b.r = {}

    def op(self, q, fn, reads=(), writes=()):
        self._deps(q, reads, writes)
        ins = fn(q.h)
        q.count += 1
        ins.then_inc(self.sems[q.key], 1)
        self._mark((q.key, q.count), reads, writes)

    def mmg(self, fns, reads=(), writes=()):
        q = self.PE
        self._deps(q, reads, writes)
        for f in fns[:-1]:
            f(q.h)
        ins = fns[-1](q.h)
        q.count += 1
        ins.then_inc(self.sems[q.key], 1)
        self._mark((q.key, q.count), reads, writes)

    def dma(self, q, out, in_, reads=(), writes=()):
        k = q.dkeys[q.dnext % len(q.dkeys)]
        q.dnext += 1
        self._wait(q, (k, self.dcount[k]))
        self._deps(q, reads, writes)
        ins = q.h.dma_start(out=out, in_=in_)
        self.dcount[k] += 16
        ins.then_inc(self.sems[k], 16)
        self._mark((k, self.dcount[k]), reads, writes)

    def collective(self, key, in_ap, out_ap, reads, writes):
        q = self.PQ
        self._deps(q, reads, writes)
        ins = q.h.collective_compute("AllGather", ALU.bypass, replica_groups=[list(range(8))],
                                     ins=[in_ap.opt()], outs=[out_ap.opt()])
        self.cccount[key] = self.cccount.get(key, 0) + 1
        ins.then_inc(self.sems[key], 1)
        self._mark((key, self.cccount[key]), reads, writes)

    def barrier(self):
        evs = [(q.key, q.count) for q in self.allq if q.count > 0]
        evs += [(k, c) for k, c in self.dcount.items() if c > 0]
        evs += [(k, c) for k, c in self.cccount.items() if c > 0]
        for q in self.allq:
            for ev in evs:
                self._wait(q, ev)


def build_program(layers=None, stages=None):
    if layers is None:
        layers = [0, 1, 2, 3] if _DBG_LAYERS is None else [int(x) for x in _DBG_LAYERS.split(",")]
    if stages is None:
        stages = "abc" if _DBG_STAGES is None else _DBG_STAGES
    nc = bass.Bass("TRN2", target_bir_lowering=False)
    dt = nc.dram_tensor

    xT_in = dt("xT", [32, 128, T], F32, kind="ExternalInput").ap()
    pos_in = dt("pos", [1, T], I32, kind="ExternalInput").ap()
    ng_in = dt("ng", [128, 24 * 32], F32, kind="ExternalInput").ap()
    gng_in = dt("gng", [128, 2 * 32], F32, kind="ExternalInput").ap()
    lng_in = dt("lng", [128, 32], F32, kind="ExternalInput").ap()
    wsT_in = dt("wsT", [128, 16 * 128], F32, kind="ExternalInput").ap()
    sgb_in = dt("sgb", [1, 16 * 128], F32, kind="ExternalInput").ap()
    NCST = 128 * 4 + 16 + 2 + 8 * 16 * 2
    rcst_in = dt("rcst", [128, 4096], F32, kind="ExternalInput").ap()
    cst_in = dt("cst", [128, NCST], F32, kind="ExternalInput").ap()
    outT = dt("outT", [32, 128, T], F32, kind="ExternalOutput").ap()

    wspecs = {}
    layer_mixer = [0, 1, 2, 0]
    for i in range(4):
        for s in range(2):
            wspecs["fi%d%d" % (i, s)] = (32, 44, 256)
            wspecs["fo%d%d" % (i, s)] = (44, 32, 128)
        m = layer_mixer[i]
        if m == 0:
            wspecs["mi%d" % i] = (32, 64, 256)
        elif m == 1:
            wspecs["mi%d" % i] = (32, 32, 256)
        else:
            wspecs["mi%d" % i] = (32, 48, 256)
        wspecs["mo%d" % i] = (32, 32, 128)
    worder = []
    for i in layers:
        worder += ["fi%d0" % i, "fo%d0" % i, "mi%d" % i, "mo%d" % i, "fi%d1" % i, "fo%d1" % i]

    if stages == "p":
        worder = ["mi%d" % layers[0]]
    sem_names = ["pe", "act", "dve", "sp", "pool"] + ["sd%d" % i for i in range(16)] + \
                ["pd%d" % i for i in range(12)] + ["cc_" + n for n in worder]

    import contextlib
    with contextlib.ExitStack() as es:
        sems = {n: es.enter_context(nc.semaphore(n)) for n in sem_names}
        P = Prog(nc, sems)
        PE, ACT, DVE, SP, PQ = P.PE, P.ACT, P.DVE, P.SP, P.PQ

        W = {}
        wsh = {}
        for n in worder:
            kcn, nb, cb = wspecs[n]
            rows = nb * 128
            wsh[n] = dt("w_" + n, [rows // 8, kcn * cb], F32, kind="ExternalInput").ap()
        for n in worder:
            kcn, nb, cb = wspecs[n]
            rows = nb * 128
            npiece = 1
            while (rows // npiece) * kcn * cb * 4 > 72 * 1024 * 1024 or (nb % npiece) != 0:
                npiece += 1
            full = dt("wg_" + n, [rows, kcn * cb], F32, kind="Internal").ap()
            fb = Buf()
            pr = rows // npiece
            ps_ = pr // 8
            for q_ in range(npiece):
                bounce = dt("wb_%s_%d" % (n, q_), [ps_, kcn * cb], F32, kind="Internal").ap()
                bb = Buf()
                P.dma(SP, out=bounce, in_=wsh[n][q_ * ps_:(q_ + 1) * ps_, :], writes=[bb])
                P.collective("cc_" + n, bounce, full[q_ * pr:(q_ + 1) * pr, :], reads=[bb], writes=[fb])
            W[n] = WT(full, fb, kcn, nb, cb)

        hT = dt("hT", [32, 128, T], F32, kind="Internal").ap()
        hbuf = [Buf() for _ in range(NTG)]
        sc = {}
        for n in ("qT", "kT", "vT", "gT", "uT"):
            sc[n] = (dt("sc_" + n, [32, 128, T], BF16, kind="Internal").ap(), Buf())

        _uid = [0]

        def U(name):
            _uid[0] += 1
            return "s%d_%s" % (_uid[0], name)
        sb = lambda name, shape, dtp: es.enter_context(nc.sbuf_tensor(U(name), shape, dtp))
        psf = es.enter_context(nc.psum_tensor("p_psf", [128, 6, 512], F32))
        psb = es.enter_context(nc.psum_tensor("p_psb", [128, 2, 1024], BF16))
        pfb = [Buf() for _ in range(6)]
        pbb = [Buf() for _ in range(2)]
        cst = sb("cst", [128, NCST], F32)
        ng = sb("ng", [128, 24 * 32], F32)
        gng = sb("gng", [128, 64], F32)
        lng = sb("lng", [128, 32], F32)
        onesf = sb("onesf", [128, 128], F32)
        onesb = sb("onesb", [128, 128], BF16)
        ident = sb("ident", [128, 128], BF16)
        pmb = sb("pmb", [128, 128], BF16)
        cbuf = Buf()
        o = 0
        c_ident = cst[:, o:o + 128]; o += 128
        c_pm = cst[:, o:o + 128]; o += 128
        c_tri = cst[:, o:o + 128]; o += 128
        c_trimask = cst[:, o:o + 128]; o += 128
        c_kd = cst[:, o:o + 16]; o += 16
        c_invf = cst[:, o:o + 2]; o += 2
        c_gmask = cst[:, o:o + 128]; o += 128
        c_gvalid = cst[:, o:o + 128]; o += 128
        assert o == NCST
        P.dma(SP, out=cst[:], in_=cst_in, writes=[cbuf])
        P.dma(SP, out=ng[:], in_=ng_in, writes=[cbuf])
        P.dma(SP, out=gng[:], in_=gng_in, writes=[cbuf])
        P.dma(SP, out=lng[:], in_=lng_in, writes=[cbuf])
        P.op(DVE, lambda h: h.memset(onesf[:], 1.0), writes=[cbuf])
        P.op(DVE, lambda h: h.memset(onesb[:], 1.0), writes=[cbuf])
        P.op(DVE, lambda h: h.tensor_copy(out=ident[:], in_=c_ident), reads=[cbuf], writes=[cbuf])
        P.op(DVE, lambda h: h.tensor_copy(out=pmb[:], in_=c_pm), reads=[cbuf], writes=[cbuf])

        for tg in range(NTG):
            P.dma(SP, out=hT[:, :, tg * TG:(tg + 1) * TG].rearrange("c p t -> p c t"),
                  in_=xT_in[:, :, tg * TG:(tg + 1) * TG].rearrange("c p t -> p c t"), writes=[hbuf[tg]])

        st = {"wn": 0, "bn": 0, "tn": 0, "hn": 0, "sn": 0}

        @contextlib.contextmanager
        def linear_scope(m):
            with contextlib.ExitStack() as ls:
                lsb = lambda name, shape, dtp: ls.enter_context(nc.sbuf_tensor(U(name), shape, dtp))
                xT = lsb("xT", [128, 32 * 512], BF16)
                actT = lsb("actT", [128, HC, 512], BF16)
                yT = lsb("yT", [128, 32, 512], BF16)
                wring = lsb("wring", [128, 3, 8192], BF16)
                hring = lsb("hring", [128, 2, 2, 512], F32)
                tmp = lsb("tmp", [128, 6, 512], F32)
                rsb = lsb("rsb", [128, 3, 512], F32)
                stage = lsb("stage", [128, 4, 2, 512] if m in (0, 2) else [128, 1, 1, 8], BF16)
                if m in (0, 2):
                    posi = lsb("posi", [128, 512], I32)
                    trig = lsb("trig", [128, 2, 512], F32)
                    trb = Buf()
                if m == 1:
                    wmT = lsb("wmT", [128, 16, 128], BF16)
                    bbt = lsb("bbt", [128, 16, 128], F32)
                    wmb = Buf()
                xTb, actb, yTb = Buf(), Buf(), Buf()
                wbuf = [Buf() for _ in range(3)]
                hsb = [Buf() for _ in range(2)]
                tb_ = [Buf() for _ in range(6)]
                rb_ = [Buf() for _ in range(3)]
                stb = [Buf() for _ in range(4)]
                xT3 = xT[:].rearrange("p (c t) -> p c t", c=32)

                def nbank():
                    b = st["bn"] % 5
                    st["bn"] += 1
                    return b
                SSB = 5

                def ntmp():
                    t_ = st["tn"] % 6
                    st["tn"] += 1
                    return t_

                def linear(w, rhs, rhs_bufs, evac):
                    for nb in range(w.nb):
                        s = st["wn"] % 3
                        st["wn"] += 1
                        P.dma(PQ, out=wring[:, s, 0:w.kc * w.cb], in_=w.ap[nb * 128:(nb + 1) * 128, :],
                              reads=[w.buf], writes=[wbuf[s]])
                        for oc in range(w.cb // 128):
                            b = nbank()
                            fns = [(lambda h, kc=kc: h.matmul(psf[:, b, :],
                                                              wring[:, s, kc * w.cb + oc * 128: kc * w.cb + (oc + 1) * 128],
                                                              rhs(kc), start=(kc == 0), stop=(kc == w.kc - 1)))
                                   for kc in range(w.kc)]
                            P.mmg(fns, reads=[wbuf[s]] + rhs_bufs, writes=[pfb[b]])
                            evac(nb, oc, b)

                def sumsq(src, src_bufs, ri):
                    for c in range(32):
                        t_ = ntmp()
                        P.op(ACT, lambda h: h.activation(out=tmp[:, t_, :], in_=src(c), func=AF.Square),
                             reads=src_bufs(c), writes=[tb_[t_]])
                        P.mmg([lambda h: h.matmul(psf[:, SSB, :], onesf[:], tmp[:, t_, :], start=(c == 0), stop=(c == 31))],
                              reads=[tb_[t_], cbuf], writes=[pfb[SSB]])
                    P.op(DVE, lambda h: h.tensor_scalar(out=rsb[:, ri, :], in0=psf[:, SSB, :], scalar1=1.0 / D, scalar2=EPS,
                                                        op0=ALU.mult, op1=ALU.add), reads=[pfb[SSB]], writes=[rb_[ri]])
                    P.op(ACT, lambda h: h.activation(out=rsb[:, ri, :], in_=rsb[:, ri, :], func=AF.Sqrt), reads=[rb_[ri]], writes=[rb_[ri]])
                    P.op(DVE, lambda h: h.reciprocal(out=rsb[:, ri, :], in_=rsb[:, ri, :]), reads=[rb_[ri]], writes=[rb_[ri]])

                def load_h(tg, c4):
                    s = st["hn"] % 2
                    st["hn"] += 1
                    P.dma(SP, out=hring[:, s, :, :], in_=hT[c4 * 2:(c4 + 1) * 2, :, tg * TG:(tg + 1) * TG].rearrange("c p t -> p c t"),
                          reads=[hbuf[tg]], writes=[hsb[s]])
                    return s

                def prenorm(tg, gi):
                    for c4 in range(16):
                        s = load_h(tg, c4)
                        for j in range(2):
                            c = c4 * 2 + j
                            t_ = ntmp()
                            P.op(ACT, lambda h: h.activation(out=tmp[:, t_, :], in_=hring[:, s, j, :], func=AF.Square),
                                 reads=[hsb[s]], writes=[tb_[t_]])
                            P.mmg([lambda h: h.matmul(psf[:, SSB, :], onesf[:], tmp[:, t_, :], start=(c == 0), stop=(c == 31))],
                                  reads=[tb_[t_], cbuf], writes=[pfb[SSB]])
                    P.op(DVE, lambda h: h.tensor_scalar(out=rsb[:, 0, :], in0=psf[:, SSB, :], scalar1=1.0 / D, scalar2=EPS,
                                                        op0=ALU.mult, op1=ALU.add), reads=[pfb[SSB]], writes=[rb_[0]])
                    P.op(ACT, lambda h: h.activation(out=rsb[:, 0, :], in_=rsb[:, 0, :], func=AF.Sqrt), reads=[rb_[0]], writes=[rb_[0]])
                    P.op(DVE, lambda h: h.reciprocal(out=rsb[:, 0, :], in_=rsb[:, 0, :]), reads=[rb_[0]], writes=[rb_[0]])
                    for c4 in range(16):
                        s = load_h(tg, c4)
                        for j in range(2):
                            c = c4 * 2 + j
                            P.op(DVE, lambda h: h.scalar_tensor_tensor(out=xT3[:, c, :], in0=hring[:, s, j, :],
                                                                       scalar=ng[:, gi * 32 + c: gi * 32 + c + 1], in1=rsb[:, 0, :],
                                                                       op0=ALU.mult, op1=ALU.mult),
                                 reads=[hsb[s], rb_[0], cbuf], writes=[xTb])

                def postnorm(tg, gi, scale, final=False):
                    sumsq(lambda c: yT[:, c, :], lambda c: [yTb], 1)
                    for c4 in range(16):
                        s = load_h(tg, c4)
                        for j in range(2):
                            c = c4 * 2 + j
                            t_ = ntmp()
                            P.op(DVE, lambda h: h.scalar_tensor_tensor(out=tmp[:, t_, :], in0=yT[:, c, :],
                                                                       scalar=ng[:, gi * 32 + c: gi * 32 + c + 1], in1=rsb[:, 1, :],
                                                                       op0=ALU.mult, op1=ALU.mult),
                                 reads=[yTb, rb_[1], cbuf], writes=[tb_[t_]])
                            P.op(DVE, lambda h: h.scalar_tensor_tensor(out=hring[:, s, j, :], in0=tmp[:, t_, :], scalar=float(scale),
                                                                       in1=hring[:, s, j, :], op0=ALU.mult, op1=ALU.add),
                                 reads=[tb_[t_], hsb[s]], writes=[hsb[s]])
                        dst = outT if final else hT
                        P.dma(SP, out=dst[c4 * 2:(c4 + 1) * 2, :, tg * TG:(tg + 1) * TG].rearrange("c p t -> p c t"),
                              in_=hring[:, s, :, :], reads=[hsb[s]], writes=[hbuf[tg]])

                def evac_y(nb, oc, b):
                    P.op(ACT, lambda h: h.activation(out=yT[:, nb, :], in_=psf[:, b, :], func=AF.Copy),
                         reads=[pfb[b]], writes=[yTb])

                def outproj(w, tg, gi, scale, final=False):
                    linear(w, lambda kc: actT[:, kc, :], [actb], evac_y)
                    postnorm(tg, gi, scale, final)

                def ffn(i, s, final=False):
                    gi = i * 6 + (0 if s == 0 else 4)
                    wi, wo = W["fi%d%d" % (i, s)], W["fo%d%d" % (i, s)]
                    hold = {}

                    def evac1(nb, oc, b):
                        if oc == 0:
                            hold["a"] = b
                            return
                        a = hold["a"]
                        t_ = ntmp()
                        P.op(ACT, lambda h: h.activation(out=tmp[:, t_, :], in_=psf[:, a, :], func=AF.Silu),
                             reads=[pfb[a]], writes=[tb_[t_]])
                        P.op(DVE, lambda h: h.tensor_tensor(out=actT[:, nb, :], in0=tmp[:, t_, :], in1=psf[:, b, :], op=ALU.mult),
                             reads=[tb_[t_], pfb[b]], writes=[actb])
                    for tg in range(NTG):
                        prenorm(tg, gi)
                        linear(wi, lambda kc: xT3[:, kc, :], [xTb], evac1)
                        outproj(wo, tg, gi + 1, 0.5, final=final)

                def build_trig(tg, col):
                    ta, tb2, tc = ntmp(), ntmp(), ntmp()
                    bl = [tb_[ta], tb_[tb2], tb_[tc], trb]
                    posf, ang, t1 = tmp[:, ta, :], tmp[:, tb2, :], tmp[:, tc, :]
                    P.dma(SP, out=posi[:], in_=pos_in[:, tg * TG:(tg + 1) * TG].partition_broadcast(128), writes=bl)
                    P.op(DVE, lambda h: h.tensor_copy(out=posf, in_=posi[:]), reads=bl, writes=bl)
                    for (di, shift) in ((1, 0.0), (0, PI / 2)):
                        P.op(DVE, lambda h: h.tensor_scalar(out=ang, in0=posf, scalar1=c_invf[:, col:col + 1], scalar2=shift,
                                                            op0=ALU.mult, op1=ALU.add), reads=bl + [cbuf], writes=bl)
                        P.op(DVE, lambda h: h.tensor_scalar(out=t1, in0=ang, scalar1=1.0 / TWO_PI, scalar2=None, op0=ALU.mult), reads=bl, writes=bl)
                        P.op(DVE, lambda h: h.tensor_copy(out=posi[:], in_=t1), reads=bl, writes=bl)
                        P.op(DVE, lambda h: h.tensor_copy(out=t1, in_=posi[:]), reads=bl, writes=bl)
                        P.op(DVE, lambda h: h.scalar_tensor_tensor(out=ang, in0=t1, scalar=-TWO_PI, in1=ang, op0=ALU.mult, op1=ALU.add), reads=bl, writes=bl)
                        P.op(DVE, lambda h: h.tensor_single_scalar(out=t1, in_=ang, scalar=PI, op=ALU.is_gt), reads=bl, writes=bl)
                        P.op(DVE, lambda h: h.scalar_tensor_tensor(out=ang, in0=t1, scalar=-TWO_PI, in1=ang, op0=ALU.mult, op1=ALU.add), reads=bl, writes=bl)
                        P.op(DVE, lambda h: h.tensor_single_scalar(out=t1, in_=ang, scalar=-PI, op=ALU.is_lt), reads=bl, writes=bl)
                        P.op(DVE, lambda h: h.scalar_tensor_tensor(out=ang, in0=t1, scalar=TWO_PI, in1=ang, op0=ALU.mult, op1=ALU.add), reads=bl, writes=bl)
                        P.op(DVE, lambda h: h.tensor_scalar(out=ang, in0=ang, scalar1=PI, scalar2=-PI, op0=ALU.min, op1=ALU.max), reads=bl, writes=bl)
                        P.op(ACT, lambda h: h.activation(out=trig[:, di, :], in_=ang, func=AF.Sin), reads=bl, writes=bl)

                def store_stage(name, chunk0, nchunks, tg, si):
                    ap, b = sc[name]
                    P.dma(SP, out=ap[chunk0:chunk0 + nchunks, :, tg * TG:(tg + 1) * TG].rearrange("c p t -> p c t"),
                          in_=stage[:, si, 0:nchunks, :], reads=[stb[si]], writes=[b])

                def nstage():
                    s_ = st["sn"] % 4
                    st["sn"] += 1
                    return s_

                def load_u(tg):
                    ap, b = sc["uT"]
                    for c4 in range(4):
                        P.dma(SP, out=actT[:, c4 * 8:(c4 + 1) * 8, :],
                              in_=ap[c4 * 8:(c4 + 1) * 8, :, tg * TG:(tg + 1) * TG].rearrange("c p t -> p c t"),
                              reads=[b], writes=[actb])

                def retention_proj(i):
                    gi = i * 6 + 2
                    w = W["mi%d" % i]
                    hold = {}
                    for tg in range(NTG):
                        cs = trig[:, 0, :]
                        sn = trig[:, 1, :]

                        def evac(nb, oc, b):
                            if nb < 32:
                                if oc == 0:
                                    hold["a"] = b
                                    return
                                a = hold["a"]
                                t0, t1, t2, t3 = ntmp(), ntmp(), ntmp(), ntmp()
                                si = nstage()
                                P.op(DVE, lambda h: h.tensor_tensor(out=tmp[:, t0, :], in0=psf[:, a, :], in1=cs, op=ALU.mult), reads=[pfb[a], trb], writes=[tb_[t0]])
                                P.op(DVE, lambda h: h.tensor_tensor(out=tmp[:, t1, :], in0=psf[:, b, :], in1=sn, op=ALU.mult), reads=[pfb[b], trb], writes=[tb_[t1]])
                                P.op(DVE, lambda h: h.tensor_tensor(out=tmp[:, t2, :], in0=psf[:, a, :], in1=sn, op=ALU.mult), reads=[pfb[a], trb], writes=[tb_[t2]])
                                P.op(DVE, lambda h: h.tensor_tensor(out=tmp[:, t3, :], in0=psf[:, b, :], in1=cs, op=ALU.mult), reads=[pfb[b], trb], writes=[tb_[t3]])
                                P.op(DVE, lambda h: h.tensor_tensor(out=stage[:, si, 0, :], in0=tmp[:, t0, :], in1=tmp[:, t1, :], op=ALU.subtract), reads=[tb_[t0], tb_[t1]], writes=[stb[si]])
                                P.op(DVE, lambda h: h.tensor_tensor(out=stage[:, si, 1, :], in0=tmp[:, t2, :], in1=tmp[:, t3, :], op=ALU.add), reads=[tb_[t2], tb_[t3]], writes=[stb[si]])
                                store_stage("qT" if nb < 16 else "kT", (nb % 16) * 2, 2, tg, si)
                            else:
                                if oc == 0:
                                    hold["si"] = nstage()
                                si = hold["si"]
                                fn = AF.Copy if nb < 48 else AF.Silu
                                P.op(ACT, lambda h: h.activation(out=stage[:, si, oc, :], in_=psf[:, b, :], func=fn), reads=[pfb[b]], writes=[stb[si]])
                                if oc == 1:
                                    store_stage("vT" if nb < 48 else "gT", (nb % 16) * 2, 2, tg, si)
                        build_trig(tg, 0)
                        prenorm(tg, gi)
                        linear(w, lambda kc: xT3[:, kc, :], [xTb], evac)

                def mixer_out(i, final=False):
                    w = W["mo%d" % i]
                    for tg in range(NTG):
                        load_u(tg)
                        outproj(w, tg, i * 6 + 3, 1.0, final=final)

                def sgu(i, final=False):
                    P.dma(SP, out=tmp[:, 0:4, :].rearrange("p a b -> p (a b)"), in_=wsT_in, writes=[tb_[0], tb_[1], tb_[2], tb_[3]])
                    for g in range(16):
                        P.op(DVE, lambda h: h.tensor_tensor(out=wmT[:, g, :], in0=tmp[:, g // 4, (g % 4) * 128:(g % 4 + 1) * 128], in1=c_trimask, op=ALU.mult),
                             reads=[tb_[g // 4], cbuf], writes=[wmb])
                    P.dma(SP, out=bbt[:].rearrange("p g t -> p (g t)"), in_=sgb_in.partition_broadcast(128), writes=[wmb])
                    gi = i * 6 + 2
                    w = W["mi%d" % i]
                    vtok = xT[:].rearrange("p (i c) -> p i c", i=4)
                    for tg in range(NTG):
                        def evac(nb, oc, b):
                            c = (nb % 16) * 2 + oc
                            if nb < 16:
                                P.op(ACT, lambda h: h.activation(out=actT[:, c, :], in_=psf[:, b, :], func=AF.Gelu), reads=[pfb[b]], writes=[actb])
                            else:
                                P.op(ACT, lambda h: h.activation(out=yT[:, c, :], in_=psf[:, b, :], func=AF.Gelu), reads=[pfb[b]], writes=[yTb])
                        prenorm(tg, gi)
                        linear(w, lambda kc: xT3[:, kc, :], [xTb], evac)
                        s1 = nbank()
                        for c in range(32):
                            P.mmg([lambda h: h.matmul(psf[:, s1, :], onesb[:], yT[:, c, :], start=(c == 0), stop=(c == 31))],
                                  reads=[yTb, cbuf], writes=[pfb[s1]])
                        for c in range(32):
                            t_ = ntmp()
                            P.op(ACT, lambda h: h.activation(out=tmp[:, t_, :], in_=yT[:, c, :], func=AF.Square), reads=[yTb], writes=[tb_[t_]])
                            P.mmg([lambda h: h.matmul(psf[:, SSB, :], onesf[:], tmp[:, t_, :], start=(c == 0), stop=(c == 31))],
                                  reads=[tb_[t_], cbuf], writes=[pfb[SSB]])
                        P.op(DVE, lambda h: h.tensor_scalar(out=rsb[:, 2, :], in0=psf[:, s1, :], scalar1=1.0 / D, scalar2=None, op0=ALU.mult),
                             reads=[pfb[s1]], writes=[rb_[2]])
                        t_ = ntmp()
                        P.op(DVE, lambda h: h.tensor_tensor(out=tmp[:, t_, :], in0=rsb[:, 2, :], in1=rsb[:, 2, :], op=ALU.mult), reads=[rb_[2]], writes=[tb_[t_]])
                        P.op(DVE, lambda h: h.scalar_tensor_tensor(out=rsb[:, 1, :], in0=psf[:, SSB, :], scalar=1.0 / D, in1=tmp[:, t_, :],
                                                                   op0=ALU.mult, op1=ALU.subtract), reads=[pfb[SSB], tb_[t_]], writes=[rb_[1]])
                        P.op(DVE, lambda h: h.tensor_scalar(out=rsb[:, 1, :], in0=rsb[:, 1, :], scalar1=EPS, scalar2=None, op0=ALU.add), reads=[rb_[1]], writes=[rb_[1]])
                        P.op(ACT, lambda h: h.activation(out=rsb[:, 1, :], in_=rsb[:, 1, :], func=AF.Sqrt), reads=[rb_[1]], writes=[rb_[1]])
                        P.op(DVE, lambda h: h.reciprocal(out=rsb[:, 1, :], in_=rsb[:, 1, :]), reads=[rb_[1]], writes=[rb_[1]])
                        for c in range(32):
                            t_ = ntmp()
                            P.op(DVE, lambda h: h.tensor_tensor(out=tmp[:, t_, :], in0=yT[:, c, :], in1=rsb[:, 2, :], op=ALU.subtract),
                                 reads=[yTb, rb_[2]], writes=[tb_[t_]])
                            P.op(DVE, lambda h: h.scalar_tensor_tensor(out=yT[:, c, :], in0=tmp[:, t_, :], scalar=lng[:, c:c + 1], in1=rsb[:, 1, :],
                                                                       op0=ALU.mult, op1=ALU.mult), reads=[tb_[t_], rb_[1], cbuf], writes=[yTb])
                        for ti in range(4):
                            for c4 in range(8):
                                pb = st["bn"] % 2
                                P.mmg([(lambda h, j=j: h.transpose(psb[:, pb, j * 128:(j + 1) * 128], yT[:, c4 * 4 + j, ti * 128:(ti + 1) * 128], ident[:]))
                                       for j in range(4)], reads=[yTb, cbuf], writes=[pbb[pb]])
                                st["bn"] += 1
                                eng = ACT if (c4 % 2 == 0) else DVE
                                if eng is ACT:
                                    P.op(ACT, lambda h: h.activation(out=vtok[:, ti, c4 * 512:(c4 + 1) * 512], in_=psb[:, pb, 0:512], func=AF.Copy),
                                         reads=[pbb[pb]], writes=[xTb])
                                else:
                                    P.op(DVE, lambda h: h.tensor_copy(out=vtok[:, ti, c4 * 512:(c4 + 1) * 512], in_=psb[:, pb, 0:512]),
                                         reads=[pbb[pb]], writes=[xTb])
                        for c in range(32):
                            g = c // 2
                            b = nbank()
                            P.mmg([(lambda h, ti=ti: h.matmul(psf[:, b, ti * 128:(ti + 1) * 128], vtok[:, ti, c * 128:(c + 1) * 128],
                                                              wmT[:, g, :], start=True, stop=True)) for ti in range(4)],
                                  reads=[xTb, wmb], writes=[pfb[b]])
                            t_ = ntmp()
                            for ti in range(4):
                                P.op(DVE, lambda h: h.tensor_tensor(out=tmp[:, t_, ti * 128:(ti + 1) * 128], in0=psf[:, b, ti * 128:(ti + 1) * 128],
                                                                    in1=bbt[:, g, :], op=ALU.add), reads=[pfb[b], wmb], writes=[tb_[t_]])
                            P.op(DVE, lambda h: h.tensor_tensor(out=actT[:, c, :], in0=tmp[:, t_, :], in1=actT[:, c, :], op=ALU.mult),
                                 reads=[tb_[t_], actb], writes=[actb])
                        outproj(W["mo%d" % i], tg, i * 6 + 3, 1.0, final=final)

                def moba_proj(i):
                    gi = i * 6 + 2
                    w = W["mi%d" % i]
                    hold = {}
                    for tg in range(NTG):
                        cs = trig[:, 0, :]
                        sn = trig[:, 1, :]
                        tcols = slice(tg * TG, (tg + 1) * TG)

                        def evac(nb, oc, b):
                            if nb < 32:
                                if oc == 0:
                                    hold["a"] = b
                                    return
                                a = hold["a"]
                                t0, t1, t2, t3 = ntmp(), ntmp(), ntmp(), ntmp()
                                si = nstage()
                                P.op(DVE, lambda h: h.tensor_tensor(out=tmp[:, t0, :], in0=psf[:, a, :], in1=cs, op=ALU.mult), reads=[pfb[a], trb], writes=[tb_[t0]])
                                P.op(DVE, lambda h: h.tensor_tensor(out=tmp[:, t1, :], in0=psf[:, b, :], in1=sn, op=ALU.mult), reads=[pfb[b], trb], writes=[tb_[t1]])
                                P.op(DVE, lambda h: h.tensor_tensor(out=tmp[:, t2, :], in0=psf[:, a, :], in1=sn, op=ALU.mult), reads=[pfb[a], trb], writes=[tb_[t2]])
                                P.op(DVE, lambda h: h.tensor_tensor(out=tmp[:, t3, :], in0=psf[:, b, :], in1=cs, op=ALU.mult), reads=[pfb[b], trb], writes=[tb_[t3]])
                                P.op(DVE, lambda h: h.tensor_tensor(out=stage[:, si, 0, :], in0=tmp[:, t0, :], in1=tmp[:, t1, :], op=ALU.subtract), reads=[tb_[t0], tb_[t1]], writes=[stb[si]])
                                P.op(DVE, lambda h: h.tensor_tensor(out=stage[:, si, 1, :], in0=tmp[:, t2, :], in1=tmp[:, t3, :], op=ALU.add), reads=[tb_[t2], tb_[t3]], writes=[stb[si]])
                                ap, bb_ = sc["qT" if nb < 16 else "kT"]
                                h0 = (nb % 16) * 2
                                for half in range(2):
                                    for hh in range(2):
                                        P.dma(SP, out=ap[h0 + hh, half * 64:(half + 1) * 64, tcols],
                                              in_=stage[hh * 64:(hh + 1) * 64, si, half, :], reads=[stb[si]], writes=[bb_])
                            else:
                                if oc == 0:
                                    hold["si"] = nstage()
                                si = hold["si"]
                                P.op(ACT, lambda h: h.activation(out=stage[:, si, oc, :], in_=psf[:, b, :], func=AF.Copy), reads=[pfb[b]], writes=[stb[si]])
                                if oc == 1:
                                    store_stage("vT", (nb % 16) * 2, 2, tg, si)
                        build_trig(tg, 1)
                        prenorm(tg, gi)
                        linear(w, lambda kc: xT3[:, kc, :], [xTb], evac)

                import types
                yield types.SimpleNamespace(ffn=ffn, retention_proj=retention_proj, mixer_out=mixer_out, sgu=sgu, moba_proj=moba_proj)


        def retention_core(i, j):
            with contextlib.ExitStack() as rs:
                rsb_ = lambda name, shape, dtp: rs.enter_context(nc.sbuf_tensor(U(name), shape, dtp))
                qh = rsb_("r_qh", [128, 2, 2, T], BF16)
                kh = rsb_("r_kh", [128, 2, 2, T], BF16)
                vh = rsb_("r_vh", [128, 2, 2, T], BF16)
                gh = rsb_("r_gh", [128, 2, 2, T], BF16)
                uh = rsb_("r_uh", [128, 2, 2, T], BF16)
                ktok = rsb_("r_ktok", [128, 16, 256], BF16)
                vtok = rsb_("r_vtok", [128, 16, 256], BF16)
                S = rsb_("r_S", [128, 2, 256], F32)
                Sb = rsb_("r_Sb", [128, 2, 256], BF16)
                sTm = rsb_("r_sTm", [128, 2, 128], BF16)
                qs = rsb_("r_qs", [128, 2, 2, 128], BF16)
                osb = rsb_("r_osb", [128, 2, 256], F32)
                onb = rsb_("r_onb", [128, 2, 256], BF16)
                stt = rsb_("r_stt", [128, 2, 8], F32)
                rc = rsb_("r_rc", [128, 4096], F32)
                rcb = Buf()
                P.dma(SP, out=rc[:], in_=rcst_in, writes=[rcb])
                c_decT = rc[:, 0:2048]
                c_qdb = rc[:, 2048:4096]
                ldb = [Buf() for _ in range(2)]
                uhb = [Buf() for _ in range(2)]
                tkb, Sbuf, sTb, qsb, osbb, onbb, sttb = Buf(), Buf(), [Buf(), Buf()], [Buf(), Buf()], [Buf(), Buf()], [Buf(), Buf()], [Buf(), Buf()]
                bn = {"n": 0}

                def nbank():
                    b = bn["n"] % 6
                    bn["n"] += 1
                    return b
                for hd in range(16):
                    sl = hd % 2
                    for (nm, dst) in (("qT", qh), ("kT", kh), ("vT", vh), ("gT", gh)):
                        ap, b = sc[nm]
                        P.dma(SP, out=dst[:, sl, :, :], in_=ap[2 * hd:2 * hd + 2, :, :].rearrange("c p t -> p c t"), reads=[b], writes=[ldb[sl]])
                    for n in range(0, 16, 2):
                        for (src, dst, isk) in ((kh, ktok, True), (vh, vtok, False)):
                            pb = bn["n"] % 2
                            bn["n"] += 1
                            P.mmg([(lambda h, q_=q_: h.transpose(psb[:, pb, q_ * 128:(q_ + 1) * 128],
                                                                 src[:, sl, q_ % 2, (n + q_ // 2) * 128:(n + q_ // 2 + 1) * 128], ident[:]))
                                   for q_ in range(4)], reads=[ldb[sl], cbuf], writes=[pbb[pb]])
                            dview = dst[:, n:n + 2, :].rearrange("p a b -> p (a b)")
                            if isk:
                                P.op(DVE, lambda h: h.tensor_scalar(out=dview, in0=psb[:, pb, 0:512], scalar1=c_kd[:, hd:hd + 1], scalar2=None, op0=ALU.mult),
                                     reads=[pbb[pb], cbuf], writes=[tkb])
                            else:
                                P.op(ACT, lambda h: h.activation(out=dview, in_=psb[:, pb, 0:512], func=AF.Copy), reads=[pbb[pb]], writes=[tkb])
                    P.op(DVE, lambda h: h.memset(S[:], 0.0), writes=[Sbuf])
                    P.op(DVE, lambda h: h.memset(Sb[:], 0.0), writes=[Sbuf])
                    cd = float(np.float32(np.exp(np.float32(np.log1p(-np.exp2(np.float32(-5.0 - hd)))) * np.float32(128.0))))
                    for n in range(16):
                        p2 = n % 2
                        cols = slice(n * 128, (n + 1) * 128)
                        b1 = nbank()
                        P.mmg([(lambda h, dc=dc: h.matmul(psf[:, b1, 0:128], kh[:, sl, dc, cols], qh[:, sl, dc, cols], start=(dc == 0), stop=(dc == 1)))
                               for dc in range(2)], reads=[ldb[sl]], writes=[pfb[b1]])
                        P.op(DVE, lambda h: h.tensor_tensor(out=sTm[:, p2, :], in0=psf[:, b1, 0:128], in1=c_decT[:, hd * 128:(hd + 1) * 128], op=ALU.mult),
                             reads=[pfb[b1], rcb], writes=[sTb[p2]])
                        for dc in range(2):
                            P.op(DVE, lambda h: h.tensor_tensor(out=qs[:, p2, dc, :], in0=qh[:, sl, dc, cols], in1=c_qdb[:, hd * 128:(hd + 1) * 128], op=ALU.mult),
                                 reads=[ldb[sl], rcb], writes=[qsb[p2]])
                        b2 = nbank()
                        fns = [lambda h: h.matmul(psf[:, b2, 0:256], sTm[:, p2, :], vtok[:, n, :], start=True, stop=False)]
                        fns += [(lambda h, dc=dc: h.matmul(psf[:, b2, 0:256], qs[:, p2, dc, :], Sb[:, dc, :], start=False, stop=(dc == 1))) for dc in range(2)]
                        P.mmg(fns, reads=[sTb[p2], tkb, qsb[p2], Sbuf], writes=[pfb[b2]])
                        P.op(ACT, lambda h: h.activation(out=osb[:, p2, :], in_=psf[:, b2, 0:256], func=AF.Copy), reads=[pfb[b2]], writes=[osbb[p2]])
                        P.op(DVE, lambda h: h.bn_stats(out=stt[:, p2, 0:6], in_=osb[:, p2, :]), reads=[osbb[p2]], writes=[sttb[p2]])
                        P.op(DVE, lambda h: h.bn_aggr(out=stt[:, p2, 6:8], in_=stt[:, p2, 0:6]), reads=[sttb[p2]], writes=[sttb[p2]])
                        P.op(DVE, lambda h: h.tensor_scalar(out=stt[:, p2, 7:8], in0=stt[:, p2, 7:8], scalar1=EPS, scalar2=None, op0=ALU.add), reads=[sttb[p2]], writes=[sttb[p2]])
                        P.op(ACT, lambda h: h.activation(out=stt[:, p2, 7:8], in_=stt[:, p2, 7:8], func=AF.Sqrt), reads=[sttb[p2]], writes=[sttb[p2]])
                        P.op(DVE, lambda h: h.reciprocal(out=stt[:, p2, 7:8], in_=stt[:, p2, 7:8]), reads=[sttb[p2]], writes=[sttb[p2]])
                        P.op(DVE, lambda h: h.tensor_scalar(out=onb[:, p2, :], in0=osb[:, p2, :], scalar1=stt[:, p2, 6:7], scalar2=stt[:, p2, 7:8],
                                                            op0=ALU.subtract, op1=ALU.mult), reads=[osbb[p2], sttb[p2]], writes=[onbb[p2]])
                        pb = bn["n"] % 2
                        bn["n"] += 1
                        P.mmg([(lambda h, dc=dc: h.transpose(psb[:, pb, dc * 128:(dc + 1) * 128], onb[:, p2, dc * 128:(dc + 1) * 128], ident[:])) for dc in range(2)],
                              reads=[onbb[p2], cbuf], writes=[pbb[pb]])
                        for dc in range(2):
                            ch = 2 * hd + dc
                            P.op(DVE, lambda h: h.scalar_tensor_tensor(out=uh[:, sl, dc, cols], in0=psb[:, pb, dc * 128:(dc + 1) * 128],
                                                                       scalar=gng[:, j * 32 + ch: j * 32 + ch + 1], in1=gh[:, sl, dc, cols],
                                                                       op0=ALU.mult, op1=ALU.mult), reads=[pbb[pb], ldb[sl], cbuf], writes=[uhb[sl]])
                        if n < 15:
                            for dc in range(2):
                                b3 = nbank()
                                P.mmg([lambda h: h.matmul(psf[:, b3, 0:256], ktok[:, n, dc * 128:(dc + 1) * 128], vtok[:, n, :], start=True, stop=True)],
                                      reads=[tkb], writes=[pfb[b3]])
                                P.op(DVE, lambda h: h.scalar_tensor_tensor(out=S[:, dc, :], in0=S[:, dc, :], scalar=cd, in1=psf[:, b3, 0:256],
                                                                           op0=ALU.mult, op1=ALU.add), reads=[pfb[b3], Sbuf], writes=[Sbuf])
                            P.op(ACT, lambda h: h.activation(out=Sb[:], in_=S[:], func=AF.Copy), reads=[Sbuf], writes=[Sbuf])
                    ap, b = sc["uT"]
                    P.dma(SP, out=ap[2 * hd:2 * hd + 2, :, :].rearrange("c p t -> p c t"), in_=uh[:, sl, :, :], reads=[uhb[sl]], writes=[b])

        def moba_core(i):
            scale = float(128 ** -0.5)
            with contextlib.ExitStack() as rs:
                msb = lambda name, shape, dtp: rs.enter_context(nc.sbuf_tensor(U(name), shape, dtp))
                qh = msb("m_qh", [128, 2, T], BF16)
                kh = msb("m_kh", [128, 2, T], BF16)
                vh = msb("m_vh", [128, 2, T], BF16)
                uh = msb("m_uh", [128, 2, T], BF16)
                vtok = msb("m_vtok", [128, 16, 128], BF16)
                kmf = msb("m_kmf", [128, 8], F32)
                kmb = msb("m_kmb", [128, 8], F32)
                qf = msb("m_qf", [128, 2, 128], F32)
                qfb = [Buf(), Buf()]
                gm = msb("m_gm", [128, 2, 32], F32)
                sm = msb("m_sm", [128, 2, T], F32)
                pm_ = msb("m_p", [128, 2, T], BF16)
                pT = msb("m_pT", [128, 2, 512], BF16)
                osb = msb("m_osb", [128, 2, 128], BF16)
                rs_ = msb("m_rs", [128, 2, 4], F32)
                ldb = [Buf() for _ in range(2)]
                uhb = [Buf() for _ in range(2)]
                vtb, kmbuf = Buf(), Buf()
                gmb, smb, pb_, pTb, osbb, rsbf = [Buf(), Buf()], [Buf(), Buf()], [Buf(), Buf()], [Buf(), Buf()], [Buf(), Buf()], [Buf(), Buf()]
                bn = {"n": 0, "t": 0}

                def nbank():
                    b = bn["n"] % 6
                    bn["n"] += 1
                    return b
                for hd in range(int(os.environ.get("K_MHEADS", "32"))):
                    sl = hd % 2
                    for (nm, dst) in (("qT", qh), ("kT", kh), ("vT", vh)):
                        ap, b = sc[nm]
                        P.dma(SP, out=dst[:, sl, :], in_=ap[hd, :, :], reads=[b], writes=[ldb[sl]])
                    for n in range(0, 16, 4):
                        pb = bn["n"] % 2
                        bn["n"] += 1
                        P.mmg([(lambda h, q_=q_: h.transpose(psb[:, pb, q_ * 128:(q_ + 1) * 128], vh[:, sl, (n + q_) * 128:(n + q_ + 1) * 128], ident[:]))
                               for q_ in range(4)], reads=[ldb[sl], cbuf], writes=[pbb[pb]])
                        P.op(ACT, lambda h: h.activation(out=vtok[:, n:n + 4, :].rearrange("p a b -> p (a b)"), in_=psb[:, pb, 0:512], func=AF.Copy),
                             reads=[pbb[pb]], writes=[vtb])
                    P.op(DVE, lambda h: h.tensor_reduce(out=kmf[:], in_=kh[:, sl, :].rearrange("p (n k) -> p n k", n=8), axis=AX.X, op=ALU.add),
                         reads=[ldb[sl]], writes=[kmbuf])
                    P.op(DVE, lambda h: h.tensor_scalar(out=kmb[:], in0=kmf[:], scalar1=1.0 / 256.0, scalar2=None, op0=ALU.mult), reads=[kmbuf], writes=[kmbuf])
                    for qt in range(16):
                        p2 = qt % 2
                        blk = qt // 2
                        nk = (qt + 1) * 128
                        qcols = slice(qt * 128, (qt + 1) * 128)
                        if blk > 0:
                            bg = nbank()
                            P.op(ACT, lambda h: h.activation(out=qf[:, p2, :], in_=qh[:, sl, qcols], func=AF.Copy), reads=[ldb[sl]], writes=[qfb[p2]])
                            P.mmg([lambda h: h.matmul(psf[:, bg, 0:8], qf[:, p2, :], kmb[:], start=True, stop=True)], reads=[qfb[p2], kmbuf], writes=[pfb[bg]])
                            P.op(DVE, lambda h: h.tensor_tensor(out=gm[:, p2, 0:8], in0=psf[:, bg, 0:8], in1=c_gmask[:, qt * 8:(qt + 1) * 8], op=ALU.add),
                                 reads=[pfb[bg], cbuf], writes=[gmb[p2]])
                            P.op(DVE, lambda h: h.max(out=gm[:, p2, 8:16], in_=gm[:, p2, 0:8]), reads=[gmb[p2]], writes=[gmb[p2]])
                            P.op(DVE, lambda h: h.tensor_scalar(out=gm[:, p2, 16:24], in0=gm[:, p2, 0:8], scalar1=gm[:, p2, 10:11], scalar2=None, op0=ALU.is_ge),
                                 reads=[gmb[p2]], writes=[gmb[p2]])
                            P.op(DVE, lambda h: h.tensor_tensor(out=gm[:, p2, 16:24], in0=gm[:, p2, 16:24], in1=c_gvalid[:, qt * 8:(qt + 1) * 8], op=ALU.mult),
                                 reads=[gmb[p2], cbuf], writes=[gmb[p2]])
                            P.op(DVE, lambda h: h.tensor_scalar(out=gm[:, p2, 24:32], in0=gm[:, p2, 16:24], scalar1=-1.0, scalar2=-NEG, op0=ALU.add, op1=ALU.mult),
                                 reads=[gmb[p2]], writes=[gmb[p2]])
                        for k0 in range(0, nk, 512):
                            kw = min(512, nk - k0)
                            bs = nbank()
                            P.mmg([lambda h: h.matmul(psf[:, bs, 0:kw], qh[:, sl, qcols], kh[:, sl, k0:k0 + kw], start=True, stop=True)],
                                  reads=[ldb[sl]], writes=[pfb[bs]])
                            for kb in range(k0, k0 + kw, 256):
                                n_ = kb // 256
                                if n_ < blk:
                                    P.op(DVE, lambda h: h.tensor_scalar(out=sm[:, p2, kb:kb + 256], in0=psf[:, bs, kb - k0:kb - k0 + 256],
                                                                        scalar1=gm[:, p2, 24 + n_:25 + n_], scalar2=None, op0=ALU.add),
                                         reads=[pfb[bs], gmb[p2]], writes=[smb[p2]])
                                else:
                                    if qt % 2 == 1:
                                        P.op(DVE, lambda h: h.tensor_copy(out=sm[:, p2, kb:kb + 128], in_=psf[:, bs, kb - k0:kb - k0 + 128]),
                                             reads=[pfb[bs]], writes=[smb[p2]])
                                        kk = kb + 128
                                    else:
                                        kk = kb
                                    P.op(DVE, lambda h: h.tensor_tensor(out=sm[:, p2, kk:kk + 128], in0=psf[:, bs, kk - k0:kk - k0 + 128], in1=c_tri, op=ALU.add),
                                         reads=[pfb[bs], cbuf], writes=[smb[p2]])
                        P.op(DVE, lambda h: h.tensor_reduce(out=rs_[:, p2, 0:1], in_=sm[:, p2, 0:nk], axis=AX.X, op=ALU.max), reads=[smb[p2]], writes=[rsbf[p2]])
                        P.op(DVE, lambda h: h.tensor_scalar(out=rs_[:, p2, 1:2], in0=rs_[:, p2, 0:1], scalar1=-scale, scalar2=None, op0=ALU.mult), reads=[rsbf[p2]], writes=[rsbf[p2]])
                        P.op(ACT, lambda h: h.activation(out=pm_[:, p2, 0:nk], in_=sm[:, p2, 0:nk], func=AF.Exp, bias=rs_[:, p2, 1:2], scale=scale,
                                                         accum_out=rs_[:, p2, 2:3]), reads=[smb[p2], rsbf[p2]], writes=[pb_[p2], rsbf[p2]])
                        P.op(DVE, lambda h: h.reciprocal(out=rs_[:, p2, 3:4], in_=rs_[:, p2, 2:3]), reads=[rsbf[p2]], writes=[rsbf[p2]])
                        bo = nbank()
                        nt = qt + 1
                        for k4 in range(0, nt, 4):
                            kn = min(4, nt - k4)
                            pb = bn["n"] % 2
                            bn["n"] += 1
                            tp = bn["t"] % 2
                            bn["t"] += 1
                            P.mmg([(lambda h, q_=q_: h.transpose(psb[:, pb, q_ * 128:(q_ + 1) * 128], pm_[:, p2, (k4 + q_) * 128:(k4 + q_ + 1) * 128], ident[:]))
                                   for q_ in range(kn)], reads=[pb_[p2], cbuf], writes=[pbb[pb]])
                            if tp == 0:
                                P.op(ACT, lambda h: h.activation(out=pT[:, tp, 0:kn * 128], in_=psb[:, pb, 0:kn * 128], func=AF.Copy), reads=[pbb[pb]], writes=[pTb[tp]])
                            else:
                                P.op(DVE, lambda h: h.tensor_copy(out=pT[:, tp, 0:kn * 128], in_=psb[:, pb, 0:kn * 128]), reads=[pbb[pb]], writes=[pTb[tp]])
                            P.mmg([(lambda h, q_=q_: h.matmul(psf[:, bo, 0:128], pT[:, tp, q_ * 128:(q_ + 1) * 128], vtok[:, k4 + q_, :],
                                                              start=(k4 + q_ == 0), stop=(k4 + q_ == nt - 1))) for q_ in range(kn)],
                                  reads=[pTb[tp], vtb], writes=[pfb[bo]])
                        P.op(DVE, lambda h: h.tensor_scalar(out=osb[:, p2, :], in0=psf[:, bo, 0:128], scalar1=rs_[:, p2, 3:4], scalar2=None, op0=ALU.mult),
                             reads=[pfb[bo], rsbf[p2]], writes=[osbb[p2]])
                        pb = bn["n"] % 2
                        bn["n"] += 1
                        P.mmg([lambda h: h.transpose(psb[:, pb, 0:128], osb[:, p2, :], ident[:])], reads=[osbb[p2], cbuf], writes=[pbb[pb]])
                        P.op(ACT, lambda h: h.activation(out=uh[:, sl, qcols], in_=psb[:, pb, 0:128], func=AF.Copy), reads=[pbb[pb]], writes=[uhb[sl]])
                    ap, b = sc["uT"]
                    P.dma(SP, out=ap[hd, :, :], in_=uh[:, sl, :], reads=[uhb[sl]], writes=[b])

        if stages == "p":
            tout = dt("t_q", [32, 128, T], BF16, kind="ExternalOutput").ap()
            P.barrier()
            with linear_scope(2) as L:
                L.moba_proj(layers[0])
                P.barrier()
            P.dma(SP, out=tout, in_=sc[os.environ.get("K_PWHICH", "qT")][0], reads=[sc["qT"][1], sc["kT"][1], sc["vT"][1]], writes=[Buf()])
            P.barrier()
            return nc
        if stages == "m":
            tin = {n: dt("t_" + n, [32, 128, T], BF16, kind="ExternalInput").ap() for n in ("qT", "kT", "vT")}
            tout = dt("t_uT", [32, 128, T], BF16, kind="ExternalOutput").ap()
            for n in ("qT", "kT", "vT"):
                P.dma(SP, out=sc[n][0], in_=tin[n], writes=[sc[n][1]])
            P.barrier()
            moba_core(2)
            P.dma(SP, out=tout, in_=sc["uT"][0], reads=[sc["uT"][1]], writes=[Buf()])
            P.barrier()
            return nc
        nlay = len(layers)
        P.barrier()
        _dump = os.environ.get("K_DUMP") is not None
        dbg = {}
        if _dump:
            for i in layers[:-1]:
                dbg[i] = dt("dbg%d" % i, [32, 128, T], F32, kind="ExternalOutput").ap()
        for li, i in enumerate(layers):
            m = layer_mixer[i]
            j = i // 3
            last_layer = (li == nlay - 1)
            do_b = "b" in stages
            do_c = "c" in stages
            if m == 1:
                with linear_scope(1) as L:
                    if "a" in stages:
                        L.ffn(i, 0, final=(last_layer and not do_b and not do_c))
                    if do_b:
                        L.sgu(i, final=(last_layer and not do_c))
                    if do_c:
                        L.ffn(i, 1, final=last_layer)
                    P.barrier()
            else:
                with linear_scope(m) as L:
                    if "a" in stages:
                        L.ffn(i, 0, final=(last_layer and not do_b and not do_c))
                    if do_b:
                        (L.retention_proj if m == 0 else L.moba_proj)(i)
                    P.barrier()
                if do_b:
                    if m == 0:
                        retention_core(i, j)
                    else:
                        moba_core(i)
                    P.barrier()
                if do_b or do_c:
                    with linear_scope(-1) as L:
                        if do_b:
                            L.mixer_out(i, final=(last_layer and not do_c))
                        if do_c:
                            L.ffn(i, 1, final=last_layer)
                        P.barrier()
            if _dump and not last_layer:
                db = Buf()
                P.dma(SP, out=dbg[i], in_=hT, reads=hbuf, writes=[db])
        P.barrier()
    return nc


def _relayout(Wm, kcn, nb, cb, perm=None):
    if perm is not None:
        Wm = Wm[:, perm]
    a = Wm.reshape(kcn, 128, nb, cb).transpose(2, 1, 0, 3).reshape(nb * 128, kcn * cb)
    return a


def _shards(Wr, nb, kcn, cb):
    rows = nb * 128
    npiece = 1
    while (rows // npiece) * kcn * cb * 4 > 72 * 1024 * 1024 or (nb % npiece) != 0:
        npiece += 1
    pr = rows // npiece
    ps_ = pr // 8
    v = Wr.reshape(npiece, 8, ps_, kcn * cb)
    return [np.ascontiguousarray(v[:, c].reshape(npiece * ps_, kcn * cb)) for c in range(8)]


def _consts():
    ident = np.eye(128, dtype=np.float32)
    pm = np.zeros((128, 128), np.float32)
    for m_ in range(64):
        pm[m_ + 64, m_] = -1.0
        pm[m_, m_ + 64] = 1.0
    qi = np.arange(128)
    tri = np.where(qi[None, :] <= qi[:, None], 0.0, NEG).astype(np.float32)
    trimask = (qi[:, None] <= qi[None, :]).astype(np.float32)
    hh = np.arange(16, dtype=np.float32)
    lg = np.log1p(-np.exp2(-5.0 - hh)).astype(np.float32)
    idx = np.arange(128, dtype=np.float32)
    kd = (np.exp(lg[None, :] * (127.0 - idx[:, None])) / 16.0).astype(np.float32)
    invf_ret = (1.0 / (10000.0 ** np.linspace(0.0, 1.0, 128, dtype=np.float32))).astype(np.float32)
    f_m = (1.0 / (10000.0 ** (np.arange(0, 128, 2, dtype=np.float32) / 128.0))).astype(np.float32)
    invf = np.stack([invf_ret, np.concatenate([f_m, f_m])], axis=1).astype(np.float32)
    gmask = np.zeros((16, 8), np.float32)
    gvalid = np.zeros((16, 8), np.float32)
    for qt in range(16):
        for n_ in range(8):
            if n_ < qt // 2:
                gvalid[qt, n_] = 1.0
            else:
                gmask[qt, n_] = NEG
    gmask = np.broadcast_to(gmask.reshape(1, 128), (128, 128))
    gvalid = np.broadcast_to(gvalid.reshape(1, 128), (128, 128))
    cst = np.concatenate([ident, pm, tri, trimask, kd, invf, gmask, gvalid], axis=1).astype(np.float32)
    dist = idx[None, :] - idx[:, None]
    decT = np.where(dist[None] >= 0, np.exp(lg[:, None, None] * np.maximum(dist[None], 0.0)), 0.0) / 16.0
    decT = decT.transpose(1, 0, 2).reshape(128, 2048)
    qd = np.exp(lg[:, None] * (idx[None, :] + 1.0))
    qdb = np.broadcast_to(qd.reshape(1, 2048), (128, 2048))
    rcst = np.concatenate([decT, qdb], axis=1).astype(np.float32)
    return np.ascontiguousarray(cst), np.ascontiguousarray(rcst)


def _moba_perm():
    cols = []
    for base in (0, D):
        for j_ in range(16):
            h0 = 2 * j_
            for half in range(2):
                for hh in range(2):
                    s0 = base + (h0 + hh) * 128 + half * 64
                    cols.append(np.arange(s0, s0 + 64))
    cols.append(np.arange(2 * D, 3 * D))
    return np.concatenate(cols)


_NC_CACHE = {}
_GROUPS = [[0, 1, 2, 3]]


def _layer_weights(i, ffn_w_in, ffn_w_out, ret_w_in, ret_w_out, sg_w_in, sg_w_out, moba_w_in, moba_w_out):
    layer_mixer = [0, 1, 2, 0]
    wsh = {}
    for s in range(2):
        wi = ffn_w_in[i, s].reshape(D, 2, HC, 128).transpose(0, 2, 1, 3).reshape(D, 2 * DFF)
        wsh["fi%d%d" % (i, s)] = _shards(_relayout(wi, 32, 44, 256), 44, 32, 256)
        wsh["fo%d%d" % (i, s)] = _shards(_relayout(ffn_w_out[i, s], 44, 32, 128), 32, 44, 128)
    m = layer_mixer[i]
    j = i // 3
    if m == 0:
        wsh["mi%d" % i] = _shards(_relayout(ret_w_in[j], 32, 64, 256), 64, 32, 256)
        wsh["mo%d" % i] = _shards(_relayout(ret_w_out[j], 32, 32, 128), 32, 32, 128)
    elif m == 1:
        wsh["mi%d" % i] = _shards(_relayout(sg_w_in[j], 32, 32, 256), 32, 32, 256)
        wsh["mo%d" % i] = _shards(_relayout(sg_w_out[j], 32, 32, 128), 32, 32, 128)
    else:
        wsh["mi%d" % i] = _shards(_relayout(moba_w_in[j][:, _moba_perm()], 32, 48, 256), 48, 32, 256)
        wsh["mo%d" % i] = _shards(_relayout(moba_w_out[j], 32, 32, 128), 32, 32, 128)
    return wsh


def kernel(x, positions, norm_g, ffn_w_in, ffn_w_out, ret_w_in, ret_gn_g, ret_w_out,
           sg_w_in, sg_ln_g, sg_w_s, sg_b, sg_w_out, moba_w_in, moba_w_out):
    f32 = lambda a: np.asarray(a, np.float32)
    x = f32(x)
    cst, rcst = _consts()
    ng = np.ascontiguousarray(f32(norm_g).reshape(24, 32, 128).transpose(2, 0, 1).reshape(128, 24 * 32))
    gng = np.ascontiguousarray(f32(ret_gn_g).reshape(2, 32, 128).transpose(2, 0, 1).reshape(128, 64))
    lng = np.ascontiguousarray(f32(sg_ln_g).reshape(32, 128).T)
    wsT = np.ascontiguousarray(f32(sg_w_s)[0].transpose(2, 0, 1).reshape(128, 16 * 128))
    sgb = np.ascontiguousarray(f32(sg_b)[0].reshape(1, 16 * 128))
    pos = np.asarray(positions).astype(np.int32)
    groups = _GROUPS if _DBG_LAYERS is None else [[int(v) for v in _DBG_LAYERS.split(",")]]
    cur = [np.ascontiguousarray(x[c // 2].T.reshape(32, 128, T)) for c in range(8)]
    for grp in groups:
        key = tuple(grp)
        if key not in _NC_CACHE:
            _NC_CACHE[key] = build_program(list(grp))
        nc = _NC_CACHE[key]
        wsh = {}
        for i in grp:
            wsh.update(_layer_weights(i, f32(ffn_w_in), f32(ffn_w_out), f32(ret_w_in), f32(ret_w_out), f32(sg_w_in),
                                      f32(sg_w_out), f32(moba_w_in), f32(moba_w_out)))
        in_maps = []
        for c in range(8):
            mp = {"xT": cur[c], "pos": np.ascontiguousarray(pos[c // 2].reshape(1, T)),
                  "ng": ng, "gng": gng, "lng": lng, "wsT": wsT, "sgb": sgb, "cst": cst, "rcst": rcst}
            for n, sh in wsh.items():
                mp["w_" + n] = sh[c]
            in_maps.append(mp)
        res = run_bass_kernel_spmd(nc, in_maps, core_ids=list(range(8)))
        cur = [np.ascontiguousarray(res.results[c]["outT"]) for c in range(8)]
        del wsh, in_maps
    out = np.empty((4, T, D), np.float32)
    for b in range(4):
        out[b] = cur[2 * b].reshape(D, T).T
    return out
```
